# Optimizing a Trainium2 kernel written in Bass

```python
import math
import jax, jax.numpy as jnp
from jax import lax
import numpy as np

D_MODEL = 1024
BATCH = 32
SEQ = 2048
DEPTH = 1

HEAD_DIM = 64
FOX_HEADS = D_MODEL // 2 // HEAD_DIM
RWKV_HEADS = D_MODEL // 2 // HEAD_DIM
FOX_WIDTH = FOX_HEADS * HEAD_DIM
RWKV_WIDTH = RWKV_HEADS * HEAD_DIM
D_MIX = FOX_WIDTH + RWKV_WIDTH
Q_BLOCK = 128
RMS_EPS = 1e-6
LNX_EPS = 64e-5
DECAY_LORA = max(32, int(round(1.8 * D_MODEL ** 0.5 / 32)) * 32)
ICLR_LORA = max(32, int(round(1.8 * D_MODEL ** 0.5 / 32)) * 32)
GATE_LORA = max(32, int(round(0.6 * D_MODEL ** 0.8 / 32)) * 32)
FOX_SPLITS = [FOX_WIDTH, FOX_WIDTH, FOX_WIDTH, FOX_WIDTH, FOX_HEADS, FOX_HEADS, FOX_HEADS]
RWKV_SPLITS = [RWKV_WIDTH, RWKV_WIDTH, RWKV_WIDTH, DECAY_LORA, ICLR_LORA, GATE_LORA]
FOX_COLS = sum(FOX_SPLITS)
RWKV_COLS = sum(RWKV_SPLITS)
D_IN_PROJ = FOX_COLS + RWKV_COLS
FORGET_BIAS_MEAN = 3.0
N_EXPERTS = 64
N_GROUPS = 8
TOPK_GROUPS = 4
TOP_K = 6
D_EXPERT = 256
D_SHARED = 256
ROUTED_SCALE = 2.5
EXPERT_BLOCK = 256

kernel_name = "hybrid_fox_rwkv7_moe_adaln"


def rms_norm(x, gain, eps=RMS_EPS):
    xf = x.astype(jnp.float32)
    y = xf * lax.rsqrt(jnp.mean(xf * xf, axis=-1, keepdims=True) + eps)
    return (y * gain.astype(jnp.float32)).astype(x.dtype)


def shift_prev(z):
    pad = [(0, 0), (1, 0)] + [(0, 0)] * (z.ndim - 2)
    return jnp.pad(z, pad)[:, :-1]


def split_cols(z, sizes):
    cuts = [int(s) for s in np.cumsum(sizes)[:-1]]
    return jnp.split(z, cuts, axis=-1)


def swiglu(x, wg, wu, wd):
    return (jax.nn.silu(x @ wg) * (x @ wu)) @ wd


def fox_attention(q, k, v, cum):
    B, T, H, D = q.shape
    nb = T // Q_BLOCK
    scale = D ** -0.5
    qb = q.reshape(B, nb, Q_BLOCK, H, D).transpose(1, 0, 3, 2, 4)
    cb = cum.reshape(B, nb, Q_BLOCK, H).transpose(1, 0, 3, 2)
    kh = k.transpose(0, 2, 1, 3)
    vh = v.transpose(0, 2, 1, 3)
    ck = cum.transpose(0, 2, 1)
    key_pos = jnp.arange(T)

    def block(args):
        q_blk, c_blk, i = args
        s = jnp.einsum('bhqd,bhkd->bhqk', q_blk, kh).astype(jnp.float32) * scale
        s = s + (c_blk[..., :, None] - ck[:, :, None, :])
        q_pos = i * Q_BLOCK + jnp.arange(Q_BLOCK)
        s = jnp.where(key_pos[None, :] <= q_pos[:, None], s, -jnp.inf)
        p = jax.nn.softmax(s, axis=-1)
        return jnp.einsum('bhqk,bhkd->bhqd', p.astype(vh.dtype), vh)

    o = lax.map(block, (qb, cb, jnp.arange(nb)))
    return o.transpose(1, 0, 3, 2, 4).reshape(B, T, H, D)


def fox_mixer(zf, qn_g, kn_g, on_g, forget_b):
    B, T, _ = zf.shape
    q, k, v, g, f_logit, a_k, a_v = split_cols(zf, FOX_SPLITS)
    heads = lambda t: t.reshape(B, T, FOX_HEADS, HEAD_DIM)
    q, k, v = heads(q), heads(k), heads(v)
    a_k = jax.nn.sigmoid(a_k)[..., None]
    a_v = jax.nn.sigmoid(a_v)[..., None]
    k = a_k * shift_prev(k) + (1 - a_k) * k
    v = a_v * shift_prev(v) + (1 - a_v) * v
    q = rms_norm(q, qn_g)
    k = rms_norm(k, kn_g)
    log_f = jax.nn.log_sigmoid((f_logit + forget_b).astype(jnp.float32))
    cum = jnp.cumsum(log_f, axis=1)
    o = fox_attention(q, k, v, cum)
    o = rms_norm(o, on_g) * jax.nn.sigmoid(heads(g))
    return o.reshape(B, T, FOX_WIDTH)


def wkv7_scan(r, w, k, v, a, b):
    B, T, H, N = r.shape

    def step(S, inp):
        r_t, w_t, k_t, v_t, a_t, b_t = inp
        Sa = jnp.einsum('bhij,bhj->bhi', S, a_t)
        S = S * w_t[:, :, None, :] + Sa[..., None] * b_t[:, :, None, :] + v_t[..., None] * k_t[:, :, None, :]
        return S, jnp.einsum('bhij,bhj->bhi', S, r_t)

    xs = tuple(t.transpose(1, 0, 2, 3) for t in (r, w, k, v, a, b))
    S0 = jnp.zeros((B, H, N, N), jnp.float32)
    _, ys = lax.scan(step, S0, xs)
    return ys.transpose(1, 0, 2, 3)


def rwkv7_mixer(zr, mu, w0, decay_up, a0, iclr_up, gate_up, k_k, k_a, r_k, lnx_g, lnx_b):
    B, T, _ = zr.shape
    f32 = jnp.float32
    zr = zr + mu * (shift_prev(zr) - zr)
    r, k, v, wd, ad, gd = split_cols(zr, RWKV_SPLITS)
    w_log = -jax.nn.softplus(-(w0 + jnp.tanh(wd) @ decay_up).astype(f32)) - 0.5
    decay = jnp.exp(-jnp.exp(w_log))
    a = jax.nn.sigmoid((a0 + ad @ iclr_up).astype(f32))
    g = jax.nn.sigmoid(gd) @ gate_up
    heads = lambda t: t.astype(f32).reshape(B, T, RWKV_HEADS, HEAD_DIM)
    kk = heads(k * k_k)
    kk = kk / jnp.maximum(jnp.sqrt(jnp.sum(kk * kk, axis=-1, keepdims=True)), 1e-12)
    k = k.astype(f32) * (1 + (a - 1) * k_a.astype(f32))
    rh, kh, vh, ah, wh = heads(r), heads(k), heads(v), heads(a), heads(decay)
    o = wkv7_scan(rh, wh, kh, vh, -kk, kk * ah)
    mean = jnp.mean(o, axis=-1, keepdims=True)
    var = jnp.mean(jnp.square(o - mean), axis=-1, keepdims=True)
    o = (o - mean) * lax.rsqrt(var + LNX_EPS)
    o = o * lnx_g.astype(f32).reshape(RWKV_HEADS, HEAD_DIM) + lnx_b.astype(f32).reshape(RWKV_HEADS, HEAD_DIM)
    o = o + jnp.sum(rh * kh * r_k.astype(f32), axis=-1, keepdims=True) * vh
    return (o.reshape(B, T, RWKV_WIDTH) * g.astype(f32)).astype(zr.dtype)


def routed_experts(h, idx, wts, w_gate, w_up, w_down):
    n_tok, d = h.shape
    n_assign = n_tok * TOP_K
    flat_e = idx.reshape(-1).astype(jnp.int32)
    order = jnp.argsort(flat_e)
    sorted_e = flat_e[order]
    counts = jnp.bincount(flat_e, length=N_EXPERTS).astype(jnp.int32)
    padded = (counts + EXPERT_BLOCK - 1) // EXPERT_BLOCK * EXPERT_BLOCK
    pad_end = jnp.cumsum(padded)
    pad_start = pad_end - padded
    grp_start = jnp.cumsum(counts) - counts
    dest = pad_start[sorted_e] + jnp.arange(n_assign, dtype=jnp.int32) - grp_start[sorted_e]
    n_blocks = -(-n_assign // EXPERT_BLOCK) + N_EXPERTS
    n_rows = n_blocks * EXPERT_BLOCK
    row_tok = jnp.zeros((n_rows,), jnp.int32).at[dest].set((order // TOP_K).astype(jnp.int32))
    row_w = jnp.zeros((n_rows,), wts.dtype).at[dest].set(wts.reshape(-1)[order])
    blk_start = jnp.arange(n_blocks, dtype=pad_end.dtype) * EXPERT_BLOCK
    blk_e = jnp.minimum(jnp.searchsorted(pad_end, blk_start, side='right'), N_EXPERTS - 1)

    def block(args):
        tok, w, e = args
        y = swiglu(h[tok], w_gate[e], w_up[e], w_down[e])
        return y * w[:, None]

    ys = lax.map(block, (row_tok.reshape(n_blocks, EXPERT_BLOCK), row_w.reshape(n_blocks, EXPERT_BLOCK), blk_e))
    return jax.ops.segment_sum(ys.reshape(n_rows, d), row_tok, num_segments=n_tok)


def moe_ffn(h, router_w, router_bias, w_gate, w_up, w_down, sw_gate, sw_up, sw_down):
    B, T, D = h.shape
    hf = h.reshape(B * T, D)
    n = hf.shape[0]
    scores = jax.nn.sigmoid((hf @ router_w).astype(jnp.float32))
    sel = scores + router_bias.astype(jnp.float32)
    grp = sel.reshape(n, N_GROUPS, N_EXPERTS // N_GROUPS)
    grp_score = jnp.sum(lax.top_k(grp, 2)[0], axis=-1)
    _, top_g = lax.top_k(grp_score, TOPK_GROUPS)
    gmask = jnp.any(top_g[..., None] == jnp.arange(N_GROUPS), axis=-2)
    emask = jnp.repeat(gmask, N_EXPERTS // N_GROUPS, axis=-1)
    _, idx = lax.top_k(jnp.where(emask, sel, -jnp.inf), TOP_K)
    wts = jnp.take_along_axis(scores, idx, axis=-1)
    wts = wts / jnp.sum(wts, axis=-1, keepdims=True) * ROUTED_SCALE
    routed = routed_experts(hf, idx, wts.astype(h.dtype), w_gate, w_up, w_down)
    shared = swiglu(hf, sw_gate, sw_up, sw_down)
    return (routed + shared).reshape(B, T, D)


def setup_inputs(seed: int = 0) -> dict:
    key = jax.random.key(seed)
    ks = iter(jax.random.split(key, 40))
    nrm = lambda shape, s: jax.random.normal(next(ks), shape, jnp.float32) * s
    gain = lambda shape: 1.0 + nrm(shape, 0.02)
    L, D = DEPTH, D_MODEL
    return {
        "x": nrm((BATCH, SEQ, D), 1.0),
        "c": nrm((BATCH, D), 1.0),
        "norm1_g": gain((L, D)),
        "norm2_g": gain((L, D)),
        "ada_w": nrm((L, D, 6 * D), 0.5 * D ** -0.5),
        "ada_b": nrm((L, 6 * D), 0.02),
        "w_in": nrm((L, D, D_IN_PROJ), D ** -0.5),
        "w_out": nrm((L, D_MIX, D), D_MIX ** -0.5),
        "fox_qn_g": gain((L, FOX_HEADS, HEAD_DIM)),
        "fox_kn_g": gain((L, FOX_HEADS, HEAD_DIM)),
        "fox_on_g": gain((L, FOX_HEADS, HEAD_DIM)),
        "fox_forget_b": FORGET_BIAS_MEAN + nrm((L, FOX_HEADS), 0.5),
        "rw_mu": jax.random.uniform(next(ks), (L, RWKV_COLS), jnp.float32),
        "rw_w0": jax.random.uniform(next(ks), (L, RWKV_WIDTH), jnp.float32, -6.0, -1.0),
        "rw_decay_up": nrm((L, DECAY_LORA, RWKV_WIDTH), 0.1 * DECAY_LORA ** -0.5),
        "rw_a0": nrm((L, RWKV_WIDTH), 0.1),
        "rw_iclr_up": nrm((L, ICLR_LORA, RWKV_WIDTH), 0.1 * ICLR_LORA ** -0.5),
        "rw_gate_up": nrm((L, GATE_LORA, RWKV_WIDTH), GATE_LORA ** -0.5),
        "rw_k_k": 0.85 + nrm((L, RWKV_WIDTH), 0.05),
        "rw_k_a": 1.0 + nrm((L, RWKV_WIDTH), 0.05),
        "rw_r_k": nrm((L, RWKV_HEADS, HEAD_DIM), 0.1),
        "rw_lnx_g": gain((L, RWKV_WIDTH)),
        "rw_lnx_b": nrm((L, RWKV_WIDTH), 0.02),
        "router_w": nrm((L, D, N_EXPERTS), D ** -0.5),
        "router_bias": nrm((L, N_EXPERTS), 0.01),
        "exp_w_gate": nrm((L, N_EXPERTS, D, D_EXPERT), D ** -0.5),
        "exp_w_up": nrm((L, N_EXPERTS, D, D_EXPERT), D ** -0.5),
        "exp_w_down": nrm((L, N_EXPERTS, D_EXPERT, D), D_EXPERT ** -0.5),
        "sh_w_gate": nrm((L, D, D_SHARED), D ** -0.5),
        "sh_w_up": nrm((L, D, D_SHARED), D ** -0.5),
        "sh_w_down": nrm((L, D_SHARED, D), D_SHARED ** -0.5),
        "final_g": gain((D,)),
    }


def reference(x, c, norm1_g, norm2_g, ada_w, ada_b, w_in, w_out,
              fox_qn_g, fox_kn_g, fox_on_g, fox_forget_b,
              rw_mu, rw_w0, rw_decay_up, rw_a0, rw_iclr_up, rw_gate_up, rw_k_k, rw_k_a, rw_r_k, rw_lnx_g, rw_lnx_b,
              router_w, router_bias, exp_w_gate, exp_w_up, exp_w_down, sh_w_gate, sh_w_up, sh_w_down, final_g):
    for l in range(DEPTH):
        mod = jax.nn.silu(c) @ ada_w[l] + ada_b[l]
        sh1, sc1, g1, sh2, sc2, g2 = jnp.split(mod[:, None, :], 6, axis=-1)
        h = rms_norm(x, norm1_g[l]) * (1 + sc1) + sh1
        z = h @ w_in[l]
        y_fox = fox_mixer(z[..., :FOX_COLS], fox_qn_g[l], fox_kn_g[l], fox_on_g[l], fox_forget_b[l])
        y_rwkv = rwkv7_mixer(z[..., FOX_COLS:], rw_mu[l], rw_w0[l], rw_decay_up[l], rw_a0[l], rw_iclr_up[l],
                             rw_gate_up[l], rw_k_k[l], rw_k_a[l], rw_r_k[l], rw_lnx_g[l], rw_lnx_b[l])
        x = x + g1 * (jnp.concatenate([y_fox, y_rwkv], axis=-1) @ w_out[l])
        h = rms_norm(x, norm2_g[l]) * (1 + sc2) + sh2
        x = x + g2 * moe_ffn(h, router_w[l], router_bias[l], exp_w_gate[l], exp_w_up[l], exp_w_down[l],
                             sh_w_gate[l], sh_w_up[l], sh_w_down[l])
    return rms_norm(x, final_g)
```

```python
import contextlib
import numpy as np
import concourse.bass as bass
import concourse.mybir as mybir
from concourse.bass_utils import run_bass_kernel_spmd

F32 = mybir.dt.float32
BF16 = mybir.dt.bfloat16
I32 = mybir.dt.int32
AF = mybir.ActivationFunctionType
ALU = mybir.AluOpType
AX = mybir.AxisListType
DTSZ = {F32: 4, BF16: 2, I32: 4}

D = 1024
FOXC = 2072
RWC = 1824
INP = 3896
NE = 64
PMAX = 2
C0 = float(np.exp(-0.5))
RMS_EPS = 1e-6
LNX_EPS = 64e-5


def _region(ap):
    sz = DTSZ[ap.dtype]
    dims = ap.ap
    pstep, pcnt = dims[0]
    off = ap.offset
    if pstep > 0:
        plo = off // pstep
        foff = off % pstep
    else:
        plo = 0
        foff = off
    lo = foff
    hi = foff
    for st, c in dims[1:]:
        if st >= 0:
            hi += st * (c - 1)
        else:
            lo += st * (c - 1)
    lob, hib, phi = lo * sz, (hi + 1) * sz, plo + pcnt
    if "PSUM" in str(ap.space):
        lob = (lob // 2048) * 2048
        hib = ((hib + 2047) // 2048) * 2048
        plo = (plo // 32) * 32
        phi = ((phi + 31) // 32) * 32
    return (ap.name, plo, phi, lob, hib)


class Sched:
    ENG = ("pe", "dve", "act", "pool", "sp")

    def __init__(self, nc):
        self.nc = nc
        self.prog = {e: [] for e in self.ENG}
        self.cnt = {}
        self.seen = {e: {} for e in self.ENG}
        self.track = {}

    def _need(self, eng, tok, waits):
        if tok is None:
            return
        s, v, snap = tok
        if eng == "pe" and s == "E_pe":
            return
        seen = self.seen[eng]
        if seen.get(s, 0) >= v:
            return
        waits[s] = max(waits.get(s, 0), v)
        merged = dict(seen)
        for k2, v2 in snap.items():
            if merged.get(k2, 0) < v2:
                merged[k2] = v2
        if merged.get(s, 0) < v:
            merged[s] = v
        self.seen[eng] = merged

    def _deps(self, eng, regs_r, regs_w, waits):
        for (name, plo, phi, lo, hi) in regs_r:
            for rec in self.track.get(name, ()):
                if rec[0] < phi and plo < rec[1] and rec[2] < hi and lo < rec[3]:
                    self._need(eng, rec[4], waits)
        for (name, plo, phi, lo, hi) in regs_w:
            for rec in self.track.get(name, ()):
                if rec[0] < phi and plo < rec[1] and rec[2] < hi and lo < rec[3]:
                    self._need(eng, rec[4], waits)
                    for t in rec[5].values():
                        self._need(eng, t, waits)

    def _commit(self, tok, regs_r, regs_w):
        for (name, plo, phi, lo, hi) in regs_r:
            lst = self.track.setdefault(name, [])
            covered = False
            for rec in lst:
                if rec[0] < phi and plo < rec[1] and rec[2] < hi and lo < rec[3]:
                    old = rec[5].get(tok[0])
                    if old is None or old[1] < tok[1]:
                        rec[5][tok[0]] = tok
                    if rec[0] <= plo and phi <= rec[1] and rec[2] <= lo and hi <= rec[3]:
                        covered = True
            if not covered:
                lst.append([plo, phi, lo, hi, None, {tok[0]: tok}])
        for (name, plo, phi, lo, hi) in regs_w:
            lst = self.track.setdefault(name, [])
            lst[:] = [r for r in lst if not (plo <= r[0] and r[1] <= phi and lo <= r[2] and r[3] <= hi)]
            lst.append([plo, phi, lo, hi, tok, {}])

    def op(self, eng, fn, reads=(), writes=()):
        rr = [_region(a) for a in reads]
        rw = [_region(a) for a in writes]
        waits = {}
        self._deps(eng, rr, rw, waits)
        s = "E_" + eng
        v = self.cnt.get(s, 0) + 1
        self.cnt[s] = v
        self.prog[eng].append((tuple(waits.items()), fn, (s, 1)))
        tok = (s, v, self.seen[eng])
        self._commit(tok, rr, rw)
        return tok

    def dma(self, q, out, in_, slot, after=()):
        rr = [] if "DRAM" in str(in_.space) else [_region(in_)]
        rw = [] if "DRAM" in str(out.space) else [_region(out)]
        waits = {}
        self._deps(q, rr, rw, waits)
        for t in after:
            self._need(q, t, waits)
        s = "D_" + slot
        v = self.cnt.get(s, 0) + 16
        self.cnt[s] = v
        self.prog[q].append((tuple(waits.items()), lambda e, o=out, i=in_: e.dma_start(out=o, in_=i), (s, 16)))
        tok = (s, v, self.seen[q])
        self._commit(tok, rr, rw)
        return tok

    def wait_tok(self, eng, tok):
        waits = {}
        self._need(eng, tok, waits)
        if waits:
            self.prog[eng].append((tuple(waits.items()), None, None))

    def emit(self):
        nc = self.nc
        names = sorted(self.cnt.keys())
        with contextlib.ExitStack() as st:
            sems = {n: st.enter_context(nc.semaphore(n)) for n in names}
            block = st.enter_context(nc.Block())

            def run(engname):
                def body(e):
                    for waits, fn, inc in self.prog[engname]:
                        for s, v in waits:
                            e.wait_ge(sems[s], v)
                        if fn is not None:
                            fn(e).then_inc(sems[inc[0]], inc[1])
                return body
            block.tensor(run("pe"))
            block.vector(run("dve"))
            block.scalar(run("act"))
            block.gpsimd(run("pool"))
            block.sync(run("sp"))


class _Stop(Exception):
    pass


def build(NB, T, stop=99):
    NBLK = T // 128
    P = min(PMAX, NBLK)
    NPH = NBLK // P
    TW = min(512, P * 128)
    NTL = P * 128 // TW
    BPT = TW // 128
    NF = 12

    nc = bass.Bass("TRN2", target_bir_lowering=False)
    dt_in = lambda name, shape: nc.dram_tensor(name, shape, F32, kind="ExternalInput").ap()
    x_d = dt_in("x", [NB, T, D])
    cT_d = dt_in("cT", [128, 8, NB])
    ada_w_d = dt_in("ada_w", [D, 6 * D])
    ada_b_d = dt_in("ada_b", [1, 6 * D])
    n1g_d = dt_in("norm1_g", [1, D])
    n2g_d = dt_in("norm2_g", [1, D])
    fg_d = dt_in("final_g", [1, D])
    w_in_d = dt_in("w_in", [D, INP])
    w_out_d = dt_in("w_out", [D, D])
    qg_d = dt_in("fox_qn_g", [1, 512])
    kg_d = dt_in("fox_kn_g", [1, 512])
    og_d = dt_in("fox_on_g", [1, 512])
    fb_d = dt_in("fox_forget_b", [1, 8])
    mu_d = dt_in("rw_mu", [1, RWC])
    mul_d = dt_in("mu_lora", [128, 3])
    w0_d = dt_in("rw_w0", [1, 512])
    a0_d = dt_in("rw_a0", [1, 512])
    dup_d = dt_in("rw_decay_up", [64, 512])
    iup_d = dt_in("rw_iclr_up", [64, 512])
    gup_d = dt_in("rw_gate_up", [160, 512])
    kk_d = dt_in("rw_k_k", [1, 512])
    ka_d = dt_in("rw_k_a", [1, 512])
    rk_d = dt_in("rw_r_k", [1, 512])
    lg_d = dt_in("rw_lnx_g", [1, 512])
    lb_d = dt_in("rw_lnx_b", [1, 512])
    rw_d = dt_in("router_w", [D, NE])
    rb_d = dt_in("router_bias", [1, NE])
    eg_d = dt_in("exp_w_gate", [NE, D, 256])
    eu_d = dt_in("exp_w_up", [NE, D, 256])
    ed_d = dt_in("exp_w_down", [NE, 256, D])
    sg_d = dt_in("sh_w_gate", [D, 256])
    su_d = dt_in("sh_w_up", [D, 256])
    sd_d = dt_in("sh_w_down", [256, D])
    out_d = nc.dram_tensor("out", [NB, T, D], F32, kind="ExternalOutput").ap()

    with contextlib.ExitStack() as st:
        sb = lambda name, shape, dt: st.enter_context(nc.sbuf_tensor(name, shape, dt))
        PS = st.enter_context(nc.psum_tensor("PS", [128, 8, 512], F32))
        PSb = PS[:].bitcast(BF16)

        import os as _os
        _pad = int(_os.environ.get("KPAD", "0"))
        if _pad:
            sb("padT", [128, _pad], F32)
        iot = sb("iot", [128, 128], I32)
        ident_b = sb("ident_b", [128, 128], BF16)
        ident_f = sb("ident_f", [128, 128], F32)
        triI_f = sb("triI_f", [128, 128], F32)
        ones_f = sb("ones_f", [128, 128], F32)
        mask2 = sb("mask2", [128, 256], BF16)
        mSL = sb("mSL", [128, 128], BF16)
        ones_b = sb("ones_b", [33, 128], BF16)
        brows = sb("brows", [33, 1032], BF16)
        c_mu = sb("c_mu", [128, 1536], BF16)
        c_kk = sb("c_kk", [128, 512], BF16)
        c_ka = sb("c_ka", [128, 512], BF16)
        c_rk = sb("c_rk", [128, 512], BF16)
        c_lg = sb("c_lg", [128, 512], BF16)
        c_lb = sb("c_lb", [128, 512], BF16)
        c_qg = sb("c_qg", [128, 512], BF16)
        c_kg = sb("c_kg", [128, 512], BF16)
        c_og = sb("c_og", [128, 512], BF16)
        mu_l = sb("mu_l", [128, 3], F32)
        omu_l = sb("omu_l", [128, 3], F32)
        lup = sb("lup", [128, 512], BF16)
        gup1 = sb("gup1", [128, 512], BF16)
        gup2 = sb("gup2", [32, 512], BF16)
        kT = sb("kT", [128, 4, T], BF16)
        Vt = sb("Vt", [128, NBLK, 8, 65], BF16)
        cumK = sb("cumK", [128, NBLK, 8], F32)
        cend = sb("cend", [128, NBLK, 8], F32)
        bia = sb("bia", [128, NBLK, 8], F32)
        Hst = sb("Hst", [128, 4, 64], BF16)
        hlast = sb("hlast", [128, 8, 1], BF16)
        acc = sb("acc", [128, P, D], F32)
        modA = sb("modA", [128, 3, D], BF16)
        cs = sb("cs", [128, 8, NB], F32)
        arena = sb("arena", [128, 16576], BF16)
        rbb_t = sb("rbb_t", [128, 64], F32)
        Ft = sb("Ft", [128, NF, 512], F32)
        Bh = sb("Bh", [128, 6, 512], BF16)
        TRb = sb("TRb", [128, 4, 4, 128], BF16)
        Dg = sb("Dg", [128, 4, 128], BF16)
        RWm = sb("RWm", [128, 12, 128], BF16)
        PTb = sb("PTb", [128, 3, 128], BF16)
        ymix = sb("ymix", [128, D], BF16)
        sm = sb("sm", [128, 256], F32)
        gpad = sb("gpad", [128, 64], F32)

        S = Sched(nc)
        F = lambda i: Ft[:, i, :]
        FP = lambda i: Ft[:, i:i + 2, :].rearrange("p a b -> p (a b)")
        B_ = lambda i: Bh[:, i, :]
        h3 = lambda ap: ap.rearrange("p (h d) -> p h d", h=8)
        bc8 = lambda ap8: ap8.unsqueeze(2).to_broadcast([128, 8, 64])

        Bh2 = Bh[:, 0:2, :]
        ymT = TRb[:, 0:2].rearrange("p a q t -> p (a q) t")
        hT = RWm[:, 0:9, :].rearrange("p a t -> p (a t)")[:, 0:1032].rearrange("p (o k t) -> p o k t", o=1, k=8)
        abst = F(6)[:, 0:256].rearrange("p (a c) -> p a c", a=2)
        cbc = FP(0).rearrange("p (k m) -> p k m", k=8)
        adw = Ft[:, 2:6, :].rearrange("p a b -> p (a b)").rearrange("p (u k c) -> p u k c", u=2, k=8)
        brow_f = Ft[0:1, 6:9, :].rearrange("p a b -> p (a b)")[:, 0:1032]
        brow_t = Ft[0:1, 9:12, :].rearrange("p a b -> p (a b)")[:, 0:1032]
        def tt(eng, out, a, b, op):
            S.op(eng, lambda e: e.tensor_tensor(out, a, b, op), reads=[a, b], writes=[out])

        def ts(eng, out, a, s1, op0, s2=None, op1=None):
            rd = [a] + [s for s in (s1, s2) if not isinstance(s, (int, float, type(None)))]
            if op1 is None:
                S.op(eng, lambda e: e.tensor_scalar(out, a, s1, None, op0), reads=rd, writes=[out])
            else:
                S.op(eng, lambda e: e.tensor_scalar(out, a, s1, s2, op0, op1), reads=rd, writes=[out])

        def stt(out, a, sc, b, op0, op1):
            rd = [a, b] + ([] if isinstance(sc, (int, float)) else [sc])
            S.op("dve", lambda e: e.scalar_tensor_tensor(out, a, sc, b, op0, op1), reads=rd, writes=[out])

        def act(out, in_, func, bias=0.0, scale=1.0, accum=None):
            rd = [in_] + [s for s in (bias, scale) if not isinstance(s, (int, float))]
            wr = [out] + ([accum] if accum is not None else [])
            if accum is None:
                S.op("act", lambda e: e.activation(out, in_, func, bias=bias, scale=scale), reads=rd, writes=wr)
            else:
                S.op("act", lambda e: e.activation(out, in_, func, bias=bias, scale=scale, accum_out=accum), reads=rd, writes=wr)

        def cp(eng, out, in_):
            if eng == "act":
                S.op("act", lambda e: e.copy(out, in_), reads=[in_], writes=[out])
            else:
                S.op(eng, lambda e: e.tensor_copy(out, in_), reads=[in_], writes=[out])

        def mm(out, lhsT, rhs, start, stop):
            S.op("pe", lambda e: e.matmul(out, lhsT=lhsT, rhs=rhs, start=start, stop=stop, skip_group_check=True),
                 reads=[lhsT, rhs], writes=[out])

        def tr(out, in_, ident):
            S.op("pe", lambda e: e.transpose(out, in_, ident), reads=[in_, ident], writes=[out])

        def red(out, in_):
            S.op("dve", lambda e: e.tensor_reduce(out, in_, axis=AX.X, op=ALU.add), reads=[in_], writes=[out])

        def recip(out, in_):
            S.op("dve", lambda e: e.reciprocal(out, in_), reads=[in_], writes=[out])

        def memset(eng, ap, val):
            S.op(eng, lambda e: e.memset(ap, val), writes=[ap])

        S.op("pool", lambda e: e.iota(iot[:], pattern=[[1, 128]], base=0, channel_multiplier=-1), writes=[iot[:]])
        sss = lambda out, op: S.op("dve", lambda e: e.tensor_single_scalar(out, iot[:], 0, op), reads=[iot[:]], writes=[out])
        sss(ident_f[:], ALU.is_equal)
        sss(ident_b[:], ALU.is_equal)
        sss(triI_f[:], ALU.is_ge)
        sss(mask2[:, 0:128], ALU.is_gt)
        sss(mask2[:, 128:256], ALU.is_ge)
        sss(mSL[:], ALU.is_lt)
        memset("dve", ones_f[:], 1.0)
        memset("dve", gpad[:], -1.0e30)
        memset("dve", ones_b[:], 1.0)
        memset("dve", brows[:], 0.0)
        memset("pool", Vt[:, :, :, 64:65], 1.0)
        S.dma("sp", brow_f[:, 0:512], w0_d[:, :], "ia")
        S.dma("sp", brow_f[:, 512:1024], a0_d[:, :], "ib")
        S.dma("sp", brow_f[:, 1024:1032], fb_d[:, :], "ic")
        cp("dve", brows[0:1, :], brow_f)
        tt("dve", brow_t, brow_f, brows[0:1, :], ALU.subtract)
        cp("dve", brows[32:33, :], brow_t)
        _u = [0]

        def uniq():
            _u[0] += 1
            return "i%d" % _u[0]

        def load_bc(dst, src, n, scale=None, stg=0):
            if n > 512:
                stage = Ft[:, stg:stg + 3, :].rearrange("p a b -> p (a b)")[:, 0:n]
            else:
                stage = F(stg)[:, 0:n]
            S.dma("sp", stage, src.partition_broadcast(128), uniq())
            if scale is None:
                cp("dve", dst, stage)
            else:
                ts("dve", dst, stage, scale, ALU.mult)
        load_bc(c_mu[:], mu_d[:, 0:1536], 1536, stg=0)
        load_bc(c_kk[:], kk_d[:, :], 512, stg=3)
        load_bc(c_ka[:], ka_d[:, :], 512, stg=4)
        load_bc(c_rk[:], rk_d[:, :], 512, stg=5)
        load_bc(c_lg[:], lg_d[:, :], 512, stg=6)
        load_bc(c_lb[:], lb_d[:, :], 512, stg=7)
        load_bc(c_qg[:], qg_d[:, :], 512, scale=0.125, stg=8)
        load_bc(c_kg[:], kg_d[:, :], 512, stg=9)
        load_bc(c_og[:], og_d[:, :], 512, stg=10)
        S.dma("sp", mu_l[:], mul_d[:, :], uniq())
        ts("dve", omu_l[:], mu_l[:], -1.0, ALU.mult, 1.0, ALU.add)
        S.dma("sp", F(0)[0:64, :], dup_d[:, :], uniq())
        S.dma("sp", F(0)[64:128, :], iup_d[:, :], uniq())
        cp("dve", lup[:], F(0))
        S.dma("sp", F(1), gup_d[0:128, :], uniq())
        cp("dve", gup1[:], F(1))
        S.dma("sp", F(2)[0:32, :], gup_d[128:160, :], uniq())
        cp("dve", gup2[:], F(2)[0:32, :])
        S.dma("sp", cs[:], cT_d[:, :, :], uniq())
        act(cs[:], cs[:], AF.Silu)
        def chk(k):
            if stop == k:
                raise _Stop()

        def compute_mod(b, g0, dst, ngain_d):
            S.op("dve", lambda e: e.tensor_copy(cbc, cs[:, :, b:b + 1].to_broadcast([128, 8, 128])),
                 reads=[cs[:]], writes=[cbc])
            cnt = 0
            for gi, slot in ((g0 + 1, 0), (g0, 1), (g0 + 2, 2)):
                for pc in range(8):
                    c0 = gi * D + pc * 128
                    bufi = cnt % 2
                    S.dma("sp", adw[:, bufi, :, :], ada_w_d[:, c0:c0 + 128].rearrange("(k p) c -> p k c", p=128), "adw%d" % bufi)
                    S.dma("sp", abst[:, 0, :], ada_b_d[:, c0:c0 + 128].partition_broadcast(128), "abst0")
                    if slot == 0:
                        S.dma("sp", abst[:, 1, :], ngain_d[:, pc * 128:(pc + 1) * 128].partition_broadcast(128), "abst1")
                    pso = PS[:, 6 + bufi, 0:128]
                    for k in range(8):
                        mm(pso, cbc[:, k, :], adw[:, bufi, k, :], k == 0, k == 7)
                    d = dst[:, slot, pc * 128:(pc + 1) * 128]
                    if slot == 0:
                        tt("dve", sm[:, 0:128], pso, abst[:, 0, :], ALU.add)
                        stt(d, sm[:, 0:128], 1.0, abst[:, 1, :], ALU.add, ALU.mult)
                    else:
                        tt("dve", d, pso, abst[:, 0, :], ALU.add)
                    cnt += 1

        w_in_sb = arena[:, 0:8 * 2072].rearrange("p (k c) -> p k c", k=8)
        ew = [arena[:, 0:6144], arena[:, 0:6144]]
        stg = [arena[:, 6144 + i * 2048:6144 + (i + 1) * 2048].bitcast(F32) for i in range(2)]
        stg = stg + [FP(0), FP(2), FP(4), FP(6)]
        NSTG = len(stg)
        h2T = arena[:, 10240:10240 + 8 * P * 128].rearrange("p (k t) -> p k t", k=8)
        assert P <= 2
        Wr = arena[:, 12288:12288 + 2 * P * 65].bitcast(F32).rearrange("p (n e) -> p n e", e=65)
        modB = arena[:, 12552:15624].rearrange("p (a d) -> p a d", a=3)
        fgb = FP(8)
        rbb = rbb_t[:]

        def load_w_in(part):
            cbase, width = (0, FOXC) if part == 0 else (FOXC, RWC)
            cnt = 0
            for k in range(8):
                for c0 in range(0, width, 512):
                    w = min(512, width - c0)
                    sl = 8 + cnt % 4
                    S.dma("sp", F(sl)[:, 0:w], w_in_d[k * 128:(k + 1) * 128, cbase + c0:cbase + c0 + w], "win%d" % (cnt % 4))
                    cp("pool", w_in_sb[:, k, c0:c0 + w], F(sl)[:, 0:w])
                    cnt += 1

        def block_A(b, n):
            nl = n % P
            par = n % 2
            chk(100 + n)
            xt = acc[:, nl, :]
            hTc = hT[:, 0]
            act(FP(0), xt, AF.Square, accum=sm[:, 200:201])
            act(sm[:, 201:202], sm[:, 200:201], AF.Sqrt, bias=RMS_EPS, scale=1.0 / D)
            recip(sm[:, 202:203], sm[:, 201:202])
            stt(FP(0), xt, sm[:, 202:203], modA[:, 0, :], ALU.mult, ALU.mult)
            tt("dve", ymix[:], FP(0), modA[:, 1, :], ALU.add)
            for k in range(8):
                tr(PSb[:, 0, k * 128:(k + 1) * 128], ymix[:, k * 128:(k + 1) * 128], ident_b[:])
            cp("act", hTc[:, :, 1:129], PSb[:, 0, :].rearrange("p (k t) -> p k t", k=8))
            if n % NBLK == 0:
                memset("pool", hTc[:, :, 0:1], 0.0)
            else:
                cp("pool", hTc[:, :, 0:1], hlast[:])
            cp("pool", hlast[:], hTc[:, :, 128:129])
            cur = lambda k: hTc[:, k, 1:129]
            prv = lambda k: hTc[:, k, 0:128]

            def proj(out, c0, w, lhs):
                for k in range(8):
                    mm(out, lhs(k), w_in_sb[:, k, c0:c0 + w], k == 0, k == 7)
            load_w_in(0)
            proj(PS[:, 1, :], 0, 512, cur)
            proj(PS[:, 2, :], 512, 512, cur)
            proj(PS[:, 3, :], 512, 512, prv)
            proj(PS[:, 4, :], 1024, 512, cur)
            proj(PS[:, 5, :], 1024, 512, prv)
            proj(PS[:, 6, :], 1536, 512, cur)
            for k in range(8):
                mm(PS[:, 7, 0:24], cur(k), w_in_sb[:, k, 2048:2072], k == 0, False)
            mm(PS[:, 7, 0:8], ones_b[0:33, :], brows[0:33, 1024:1032], False, True)
            act(sm[:, 0:24], PS[:, 7, 0:24], AF.Exp, scale=-1.0)
            ts("dve", sm[:, 0:24], sm[:, 0:24], 1.0, ALU.add)
            act(sm[:, 24:32], sm[:, 0:8], AF.Ln)
            recip(sm[:, 32:48], sm[:, 8:24])
            ts("dve", sm[:, 48:56], sm[:, 24:32], -1.0, ALU.mult)
            mm(PS[:, 7, 32:40], triI_f[:], sm[:, 48:56], True, True)
            mm(PS[:, 7, 40:48], ones_f[:], sm[:, 48:56], True, True)
            sn = n % NBLK
            if sn == 0:
                cp("dve", cumK[:, sn, :], PS[:, 7, 32:40])
                cp("dve", cend[:, sn, :], PS[:, 7, 40:48])
            else:
                tt("dve", cumK[:, sn, :], PS[:, 7, 32:40], cend[:, sn - 1, :], ALU.add)
                tt("dve", cend[:, sn, :], PS[:, 7, 40:48], cend[:, sn - 1, :], ALU.add)
            cp("act", F(2), PS[:, 2, :])
            tt("dve", F(3), PS[:, 3, :], F(2), ALU.subtract)
            tt("dve", h3(F(3)), h3(F(3)), bc8(sm[:, 32:40]), ALU.mult)
            tt("dve", F(2), F(3), F(2), ALU.add)
            cp("act", F(4), PS[:, 4, :])
            tt("dve", F(3), PS[:, 5, :], F(4), ALU.subtract)
            tt("dve", h3(F(3)), h3(F(3)), bc8(sm[:, 40:48]), ALU.mult)
            tt("dve", Vt[:, sn, :, 0:64], h3(F(3)), h3(F(4)), ALU.add)
            act(F(5), PS[:, 1, :], AF.Square)
            red(sm[:, 56:64], h3(F(5)))
            act(F(6), F(2), AF.Square)
            red(sm[:, 64:72], h3(F(6)))
            act(sm[:, 72:88], sm[:, 56:72], AF.Sqrt, bias=RMS_EPS, scale=1.0 / 64)
            recip(sm[:, 88:104], sm[:, 72:88])
            tt("dve", h3(F(5)), h3(PS[:, 1, :]), bc8(sm[:, 88:96]), ALU.mult)
            tt("dve", B_(0), F(5), c_qg[:], ALU.mult)
            tt("dve", h3(F(6)), h3(F(2)), bc8(sm[:, 96:104]), ALU.mult)
            tt("dve", B_(1), F(6), c_kg[:], ALU.mult)
            for hp in range(4):
                tr(PSb[:, 0, hp * 128:(hp + 1) * 128], B_(0)[:, hp * 128:(hp + 1) * 128], ident_b[:])
                tr(PSb[:, 0, 512 + hp * 128:512 + (hp + 1) * 128], B_(1)[:, hp * 128:(hp + 1) * 128], ident_b[:])
            cp("act", B_(2), PSb[:, 0, 0:512])
            cp("act", kT[:, :, sn * 128:(sn + 1) * 128], PSb[:, 0, 512:1024].rearrange("p (k t) -> p k t", k=4))
            act(B_(3), PS[:, 6, :], AF.Sigmoid)
            chk(3)
            S.op("dve", lambda e: e.tensor_tensor(bia[:, 0:sn + 1, :], cend[:, sn:sn + 1, :].to_broadcast([128, sn + 1, 8]),
                                                  cumK[:, 0:sn + 1, :], ALU.subtract),
                 reads=[cend[:, sn:sn + 1, :], cumK[:, 0:sn + 1, :]], writes=[bia[:, 0:sn + 1, :]])
            qT = B_(2)
            cnt = 0
            for h in range(8):
                hp, po = h // 2, (h % 2) * 64
                ob = PS[:, 3 + h % 2, 0:65]
                for j in range(sn + 1):
                    sps = PS[:, 1 + cnt % 2, 0:128]
                    mm(sps, kT[po:po + 64, hp, j * 128:(j + 1) * 128], qT[po:po + 64, hp * 128:(hp + 1) * 128], True, True)
                    pt = PTb[:, cnt % 3, :]
                    act(pt, sps, AF.Exp, bias=bia[:, j, h:h + 1])
                    if j == sn:
                        tt("pool", pt, pt, mask2[:, 128:256], ALU.mult)
                    mm(ob, pt, Vt[:, j, h, :], j == 0, j == sn)
                    cnt += 1
                recip(sm[:, 104:105], ob[:, 64:65])
                ts("dve", F(7)[:, h * 64:(h + 1) * 64], ob[:, 0:64], sm[:, 104:105], ALU.mult)
            act(F(5), F(7), AF.Square)
            red(sm[:, 56:64], h3(F(5)))
            act(sm[:, 72:80], sm[:, 56:64], AF.Sqrt, bias=RMS_EPS, scale=1.0 / 64)
            recip(sm[:, 88:96], sm[:, 72:80])
            tt("dve", h3(F(7)), h3(F(7)), bc8(sm[:, 88:96]), ALU.mult)
            tt("dve", F(7), F(7), c_og[:], ALU.mult)
            tt("dve", ymix[:, 0:512], F(7), B_(3), ALU.mult)
            chk(4)
            chk(200 + n)
            load_w_in(1)
            R0 = 0
            proj(PS[:, 6, :], R0, 512, cur)
            proj(PS[:, 7, :], R0, 512, prv)
            proj(PS[:, 1, :], R0 + 512, 512, cur)
            proj(PS[:, 2, :], R0 + 512, 512, prv)
            proj(PS[:, 3, :], R0 + 1024, 512, cur)
            proj(PS[:, 4, :], R0 + 1024, 512, prv)
            for mt, msz in ((0, 128), (1, 128), (2, 32)):
                c0 = R0 + 1536 + mt * 128
                for k in range(8):
                    mm(PS[0:msz, 5, mt * 128:(mt + 1) * 128], w_in_sb[:, k, c0:c0 + msz], cur(k), k == 0, k == 7)
                for k in range(8):
                    mm(PS[0:msz, 0, mt * 128:(mt + 1) * 128], w_in_sb[:, k, c0:c0 + msz], prv(k), k == 0, k == 7)
            for (pc, pp, dst, mo) in ((6, 7, 0, 0), (1, 2, 2, 512), (3, 4, 3, 1024)):
                cp("act", F(dst), PS[:, pc, :])
                tt("dve", F(1), PS[:, pp, :], F(dst), ALU.subtract)
                tt("pool", F(1), F(1), c_mu[:, mo:mo + 512], ALU.mult)
                tt("pool", F(dst), F(1), F(dst), ALU.add)
            for mt, msz in ((0, 128), (1, 128), (2, 32)):
                sl = slice(mt * 128, (mt + 1) * 128)
                ts("dve", F(4)[0:msz, sl], PS[0:msz, 5, sl], omu_l[0:msz, mt:mt + 1], ALU.mult)
                stt(F(4)[0:msz, sl], PS[0:msz, 0, sl], mu_l[0:msz, mt:mt + 1], F(4)[0:msz, sl], ALU.mult, ALU.add)
            lab = B_(0)
            act(lab[0:64, 0:128], F(4)[0:64, 0:128], AF.Tanh)
            cp("act", lab[64:128, 0:128], F(4)[64:128, 0:128])
            act(lab[:, 128:256], F(4)[:, 128:256], AF.Sigmoid)
            act(lab[0:32, 256:384], F(4)[0:32, 256:384], AF.Sigmoid)
            mm(PS[:, 5, :], lab[0:64, 0:128], lup[0:64, :], True, False)
            mm(PS[:, 5, :], ones_b[0:33, :], brows[0:33, 0:512], False, True)
            mm(PS[:, 0, :], lab[64:128, 0:128], lup[64:128, :], True, False)
            mm(PS[:, 0, :], ones_b[0:33, :], brows[0:33, 512:1024], False, True)
            mm(PS[:, 6, :], lab[:, 128:256], gup1[:], True, False)
            mm(PS[:, 6, :], lab[0:32, 256:384], gup2[0:32, :], False, True)
            act(F(5), PS[:, 5, :], AF.Sigmoid)
            act(F(6), PS[:, 0, :], AF.Sigmoid)
            cp("act", F(7), PS[:, 6, :])
            tt("dve", F(8), F(2), c_kk[:], ALU.mult)
            act(F(9), F(8), AF.Square)
            red(sm[:, 112:120], h3(F(9)))
            act(sm[:, 120:128], sm[:, 112:120], AF.Sqrt)
            ts("dve", sm[:, 120:128], sm[:, 120:128], 1e-12, ALU.max)
            recip(sm[:, 128:136], sm[:, 120:128])
            tt("dve", h3(F(8)), h3(F(8)), bc8(sm[:, 128:136]), ALU.mult)
            tt("dve", F(9), F(8), F(6), ALU.mult)
            stt(F(10), F(6), -1.0, c_ka[:], ALU.add, ALU.mult)
            tt("dve", F(10), F(10), F(2), ALU.mult)
            tt("dve", F(2), F(10), F(2), ALU.add)
            tt("dve", F(10), F(0), F(2), ALU.mult)
            tt("dve", F(10), F(10), c_rk[:], ALU.mult)
            red(sm[:, 136:144], h3(F(10)))
            mm(PS[:, 7, :], triI_f[:], F(5), True, True)
            mm(PS[:, 1, :], ones_f[:], F(5), True, True)
            act(F(11), PS[:, 7, :], AF.Exp, scale=-C0)
            tt("dve", B_(1), F(0), F(11), ALU.mult)
            act(F(11), PS[:, 7, :], AF.Exp, scale=C0)
            tt("dve", B_(2), F(2), F(11), ALU.mult)
            tt("dve", B_(3), F(9), F(11), ALU.mult)
            tt("dve", F(10), PS[:, 7, :], F(5), ALU.subtract)
            act(F(10), F(10), AF.Exp, scale=-C0)
            stt(B_(4), F(8), -1.0, F(10), ALU.mult, ALU.mult)
            cp("act", F(10), PS[:, 1, :])
            tt("dve", F(11), F(10), PS[:, 7, :], ALU.subtract)
            act(F(11), F(11), AF.Exp, scale=-C0)
            tt("dve", F(9), F(9), F(11), ALU.mult)
            tt("dve", F(8), F(2), F(11), ALU.mult)
            act(F(10), F(10), AF.Exp, scale=-C0)
            for hp in range(4):
                tt("dve", Dg[:, hp, :], ident_f[:], F(10)[:, hp * 128:(hp + 1) * 128], ALU.mult)
            cp("act", B_(5), F(3))
            for qi, src in ((0, B_(4)), (1, B_(1)), (2, B_(3)), (3, B_(2))):
                for hp in range(4):
                    tr(PSb[:, 2 + hp // 2, ((hp % 2) * 4 + qi) * 128:((hp % 2) * 4 + qi + 1) * 128], src[:, hp * 128:(hp + 1) * 128], ident_b[:])
            cp("act", TRb[:, 0:2].rearrange("p a q t -> p (a q t)"), PSb[:, 2, :])
            cp("dve", TRb[:, 2:4].rearrange("p a q t -> p (a q t)"), PSb[:, 3, :])
            cp("act", Bh2[:, 0, :], F(9))
            cp("act", Bh2[:, 1, :], F(8))
            for h in range(8):
                hp, po = h // 2, (h % 2) * 64
                AT = TRb[po:po + 64, hp, 0, :]
                RT = TRb[po:po + 64, hp, 1, :]
                BT = TRb[po:po + 64, hp, 2, :]
                KT = TRb[po:po + 64, hp, 3, :]
                ART = TRb[po:po + 64, hp, 0:2, :].rearrange("p a t -> p (a t)")
                vb = B_(5)[:, h * 64:(h + 1) * 64]
                Hh = Hst[po:po + 64, hp, :]
                mm(PS[:, 1, 0:256], BT, ART, True, True)
                mm(PS[:, 2, 0:256], KT, ART, True, True)
                mm(PS[:, 3, 0:128], AT, BT, True, True)
                M1 = RWm[:, 0:2, :].rearrange("p a t -> p (a t)")
                M2 = RWm[:, 2:4, :].rearrange("p a t -> p (a t)")
                tt("dve", M1, PS[:, 1, 0:256], mask2[:], ALU.mult)
                tt("dve", M2, PS[:, 2, 0:256], mask2[:], ALU.mult)
                tt("dve", RWm[:, 4, :], PS[:, 3, 0:128], mSL[:], ALU.mult)
                LT, QbT, MT, QkT = RWm[:, 0, :], RWm[:, 1, :], RWm[:, 2, :], RWm[:, 3, :]
                Pm, PTm = RWm[:, 4, :], LT
                TTm = RWm[:, 9, :]
                tt("pool", TTm, ident_b[:], LT, ALU.add)
                for lvl in range(1, 7):
                    Pn = RWm[:, 5 + (lvl % 2), :]
                    PTn = RWm[:, 7 + (lvl % 2), :]
                    mm(PS[:, 1, 0:128], PTm, Pm, True, True)
                    if lvl < 6:
                        mm(PS[:, 2, 0:128], Pm, PTm, True, True)
                    cp("act", Pn, PS[:, 1, 0:128])
                    if lvl < 6:
                        cp("dve", PTn, PS[:, 2, 0:128])
                    mm(PS[:, 3, 0:128], Pn, TTm, True, True)
                    TTn = RWm[:, 9 + (lvl % 2), :]
                    tt("dve", TTn, PS[:, 3, 0:128], TTm, ALU.add)
                    Pm, PTm, TTm = Pn, PTn, TTn
                Xb = RWm[:, 11, 0:64]
                Ub = RWm[:, 11, 64:128]
                mm(PS[:, 4, 0:64], AT, Hh, True, False)
                mm(PS[:, 4, 0:64], MT, vb, False, True)
                cp("act", Xb, PS[:, 4, 0:64])
                mm(PS[:, 4, 64:128], TTm, Xb, True, True)
                cp("act", Ub, PS[:, 4, 64:128])
                yo = PS[:, 5, h * 64:(h + 1) * 64]
                mm(yo, RT, Hh, True, False)
                mm(yo, QbT, Ub, False, False)
                mm(yo, QkT, vb, False, True)
                hps = PS[:, 6, h * 64:(h + 1) * 64]
                mm(hps, Dg[:, hp, :], Hst[:, hp, :], True, False)
                mm(hps, Bh2[:, 0, hp * 128:(hp + 1) * 128], Ub, False, False)
                mm(hps, Bh2[:, 1, hp * 128:(hp + 1) * 128], vb, False, True)
                cp("act", Hh, hps[po:po + 64, :])
            cp("act", F(1), PS[:, 5, :])
            red(sm[:, 152:160], h3(F(1)))
            act(F(9), F(1), AF.Square)
            red(sm[:, 160:168], h3(F(9)))
            ts("dve", sm[:, 152:160], sm[:, 152:160], 1.0 / 64, ALU.mult)
            tt("dve", sm[:, 168:176], sm[:, 152:160], sm[:, 152:160], ALU.mult)
            stt(sm[:, 160:168], sm[:, 160:168], 1.0 / 64, sm[:, 168:176], ALU.mult, ALU.subtract)
            act(sm[:, 176:184], sm[:, 160:168], AF.Sqrt, bias=LNX_EPS)
            recip(sm[:, 184:192], sm[:, 176:184])
            tt("dve", h3(F(1)), h3(F(1)), bc8(sm[:, 152:160]), ALU.subtract)
            tt("dve", h3(F(1)), h3(F(1)), bc8(sm[:, 184:192]), ALU.mult)
            tt("dve", F(1), F(1), c_lg[:], ALU.mult)
            tt("dve", F(1), F(1), c_lb[:], ALU.add)
            tt("dve", h3(F(9)), h3(F(3)), bc8(sm[:, 136:144]), ALU.mult)
            tt("dve", F(1), F(1), F(9), ALU.add)
            tt("dve", ymix[:, 512:1024], F(1), F(7), ALU.mult)
            chk(300 + n)
            for k in range(8):
                tr(PSb[:, 0, k * 128:(k + 1) * 128], ymix[:, k * 128:(k + 1) * 128], ident_b[:])
            cp("act", TRb[:, 0:2].rearrange("p a q t -> p (a q t)"), PSb[:, 0, :])
            for hf in range(2):
                wv = Ft[:, 4 + 4 * hf:8 + 4 * hf, :].rearrange("p a b -> p (a b)").bitcast(BF16).rearrange("p (k c) -> p k c", k=8)
                for k in range(8):
                    S.dma("sp", F(2 + k % 2), w_out_d[k * 128:(k + 1) * 128, hf * 512:(hf + 1) * 512], "wos%d" % (k % 2))
                    cp("pool", wv[:, k, :], F(2 + k % 2))
                for k in range(8):
                    mm(PS[:, 1 + hf, :], ymT[:, k, :], wv[:, k, :], k == 0, k == 7)
                tt("dve", F(hf), PS[:, 1 + hf, :], modA[:, 2, hf * 512:(hf + 1) * 512], ALU.mult)
                tt("dve", acc[:, nl, hf * 512:(hf + 1) * 512], F(hf), acc[:, nl, hf * 512:(hf + 1) * 512], ALU.add)

        def phase_B(b, ph):
            rw2 = FP(10)[0:64, :].rearrange("p (c e) -> p c e", c=16)
            S.dma("sp", rw2, rw_d[:, :].rearrange("(c p) e -> p c e", p=64), "pbs0")
            S.dma("sp", rbb, rb_d[:, :].partition_broadcast(128), "pbs1")
            S.dma("sp", FP(8), fg_d[:, :].partition_broadcast(128), "pbs2")
            compute_mod(b, 3, modB, n2g_d)
            memset("dve", Wr[:, :, 64:65], 1.0)
            chk(61)
            for nl in range(P):
                x2 = acc[:, nl, :]
                act(FP(0), x2, AF.Square, accum=sm[:, 200:201])
                act(sm[:, 201:202], sm[:, 200:201], AF.Sqrt, bias=RMS_EPS, scale=1.0 / D)
                recip(sm[:, 202:203], sm[:, 201:202])
                stt(FP(0), x2, sm[:, 202:203], modB[:, 0, :], ALU.mult, ALU.mult)
                tt("dve", FP(2), FP(0), modB[:, 1, :], ALU.add)
                cp("act", ymix[:], FP(2))
                for k in range(8):
                    tr(PSb[:, 0, k * 128:(k + 1) * 128], ymix[:, k * 128:(k + 1) * 128], ident_b[:])
                cp("dve", h2T[:, :, nl * 128:(nl + 1) * 128], PSb[:, 0, :].rearrange("p (k t) -> p k t", k=8))
                for c in range(16):
                    tr(PS[0:64, 1 + c // 4, (c % 4) * 128:(c % 4 + 1) * 128], FP(2)[:, c * 64:(c + 1) * 64], ident_f[:])
                for a in range(4):
                    cp("act", F(4 + a)[0:64, :], PS[0:64, 1 + a, :])
                for c in range(16):
                    mm(PS[:, 5, 0:64], F(4 + c // 4)[0:64, (c % 4) * 128:(c % 4 + 1) * 128], rw2[:, c, :], c == 0, c == 15)
                sc = sm[:, 0:64]
                sel = sm[:, 64:128]
                act(sc, PS[:, 5, 0:64], AF.Sigmoid)
                tt("dve", sel, sc, rbb, ALU.add)
                chk(62)
                sel3 = sel.rearrange("p (g e) -> p g e", g=8)
                gs = sm[:, 128:136]
                m1 = sm[:, 208:216]
                S.op("dve", lambda e: e.tensor_reduce(m1, sel3, axis=AX.X, op=ALU.max), reads=[sel], writes=[m1])
                eq = F(6)[:, 0:64].rearrange("p (g e) -> p g e", g=8)
                tt("dve", eq, sel3, m1.unsqueeze(2).to_broadcast([128, 8, 8]), ALU.is_equal)
                stt(eq, eq, -1.0e4, sel3, ALU.mult, ALU.add)
                S.op("dve", lambda e: e.tensor_reduce(gs, eq, axis=AX.X, op=ALU.max), reads=[F(6)[:, 0:64]], writes=[gs])
                tt("dve", gs, gs, m1, ALU.add)
                cp("dve", gpad[:, 0:8], gs)
                S.op("dve", lambda e: e.max(sm[:, 136:144], gpad[:]), reads=[gpad[:]], writes=[sm[:, 136:144]])
                ts("dve", sm[:, 144:152], gs, sm[:, 139:140], ALU.is_ge)
                selm = F(6)[:, 64:128]
                stt(selm.rearrange("p (g e) -> p g e", g=8), sel.rearrange("p (g e) -> p g e", g=8), 2.0,
                    sm[:, 144:152].unsqueeze(2).to_broadcast([128, 8, 8]), ALU.add, ALU.mult)
                S.op("dve", lambda e: e.max(sm[:, 152:160], selm), reads=[selm], writes=[sm[:, 152:160]])
                ts("dve", F(6)[:, 128:192], selm, sm[:, 157:158], ALU.is_ge)
                tt("dve", F(6)[:, 192:256], sc, F(6)[:, 128:192], ALU.mult)
                red(sm[:, 160:161], F(6)[:, 192:256])
                recip(sm[:, 161:162], sm[:, 160:161])
                ts("dve", sm[:, 161:162], sm[:, 161:162], 2.5, ALU.mult)
                ts("dve", Wr[:, nl, 0:64], F(6)[:, 192:256], sm[:, 161:162], ALU.mult)
            chk(6)
            dcnt = 0
            scnt = 0
            for e in range(NE + 1):
                ewb = ew[e % 2]
                gu = ewb[:, 0:4096].rearrange("p (k c) -> p k c", k=8)
                dn = ewb[:, 4096:6144].rearrange("p (k c) -> p k c", k=2)
                gsrc = eg_d[e] if e < NE else sg_d
                usrc = eu_d[e] if e < NE else su_d
                dsrc = ed_d[e] if e < NE else sd_d
                for (src, off) in ((gsrc, 0), (usrc, 256)):
                    for kh in range(2):
                        sgb = stg[scnt % NSTG]
                        S.dma("sp", sgb.rearrange("p (k c) -> p k c", k=4),
                              src[kh * 512:(kh + 1) * 512, :].rearrange("(k p) c -> p k c", p=128), "stg%d" % (scnt % NSTG))
                        cp("act" if off == 0 else "dve", gu[:, kh * 4:(kh + 1) * 4, off:off + 256], sgb.rearrange("p (k c) -> p k c", k=4))
                        scnt += 1
                for kc in range(2):
                    sgb = stg[scnt % NSTG]
                    S.dma("sp", sgb, dsrc[kc * 128:(kc + 1) * 128, :], "stg%d" % (scnt % NSTG))
                    tt("pool", dn[:, kc, :], sgb, modB[:, 2, :], ALU.mult)
                    scnt += 1
                for tl in range(NTL):
                    aT = Bh[:, 2:4, 0:TW]
                    for c in range(2):
                        for k in range(8):
                            mm(PS[:, 2 * c, 0:TW], gu[:, k, c * 128:(c + 1) * 128], h2T[:, k, tl * TW:(tl + 1) * TW], k == 0, k == 7)
                        for k in range(8):
                            mm(PS[:, 2 * c + 1, 0:TW], gu[:, k, 256 + c * 128:256 + (c + 1) * 128], h2T[:, k, tl * TW:(tl + 1) * TW], k == 0, k == 7)
                        act(Bh[:, c, 0:TW], PS[:, 2 * c, 0:TW], AF.Silu)
                        tt("dve", aT[:, c, :], Bh[:, c, 0:TW], PS[:, 2 * c + 1, 0:TW], ALU.mult)
                    for bk in range(BPT):
                        nl = tl * BPT + bk
                        pb = 4 + 2 * (dcnt % 2)
                        for hf in range(2):
                            for c in range(2):
                                mm(PS[:, pb + hf, :], aT[:, c, bk * 128:(bk + 1) * 128], dn[:, c, hf * 512:(hf + 1) * 512], c == 0, c == 1)
                        for hf in range(2):
                            stt(acc[:, nl, hf * 512:(hf + 1) * 512], PS[:, pb + hf, :], Wr[:, nl, e:e + 1],
                                acc[:, nl, hf * 512:(hf + 1) * 512], ALU.mult, ALU.add)
                        dcnt += 1
            toks = []
            for nl in range(P):
                n = ph * P + nl
                x3 = acc[:, nl, :]
                act(FP(0), x3, AF.Square, accum=sm[:, 200:201])
                act(sm[:, 201:202], sm[:, 200:201], AF.Sqrt, bias=RMS_EPS, scale=1.0 / D)
                recip(sm[:, 202:203], sm[:, 201:202])
                ob = FP(2 + 2 * (nl % 2))
                stt(ob, x3, sm[:, 202:203], fgb, ALU.mult, ALU.mult)
                toks.append(S.dma("sp", out_d[b, n * 128:(n + 1) * 128, :], ob, "ost%d" % (nl % 2)))
            return toks

        out_toks = []
        try:
            for b in range(NB):
                memset("dve", Hst[:], 0.0)
                for ph in range(NPH):
                    for nl in range(P):
                        n = ph * P + nl
                        S.dma("sp", acc[:, nl, :], x_d[b, n * 128:(n + 1) * 128, :], "xld%d" % nl)
                    chk(1)
                    if ph == 0:
                        compute_mod(b, 0, modA, n1g_d)
                    chk(2)
                    for nl in range(P):
                        block_A(b, ph * P + nl)
                    chk(5)
                    out_toks = phase_B(b, ph)
        except _Stop:
            cp("dve", FP(0), acc[:, 0, :])
            out_toks = [S.dma("sp", out_d[0, 0:128, :], FP(0), "dbgout")]
        for t in out_toks:
            S.wait_tok("sp", t)
        S.emit()
        print("sbuf bytes remaining", nc.sbuf_bytes_remaining, "instr", {k: len(v) for k, v in S.prog.items()})
    return nc


_CACHE = {}


def _core_inputs(inp, b0, NB):
    g = lambda k: np.ascontiguousarray(np.asarray(inp[k], dtype=np.float32))
    c = g("c")[b0:b0 + NB]
    cT = np.ascontiguousarray(c.T.reshape(8, 128, NB).transpose(1, 0, 2))
    mu = g("rw_mu")[0]
    mul = np.zeros((128, 3), np.float32)
    mul[:, 0] = mu[1536:1664]
    mul[:, 1] = mu[1664:1792]
    mul[:32, 2] = mu[1792:1824]
    m = {
        "x": np.ascontiguousarray(g("x")[b0:b0 + NB]),
        "cT": cT,
        "ada_w": g("ada_w")[0], "ada_b": g("ada_b"),
        "norm1_g": g("norm1_g"), "norm2_g": g("norm2_g"), "final_g": g("final_g").reshape(1, D),
        "w_in": g("w_in")[0], "w_out": g("w_out")[0],
        "fox_qn_g": g("fox_qn_g").reshape(1, 512), "fox_kn_g": g("fox_kn_g").reshape(1, 512),
        "fox_on_g": g("fox_on_g").reshape(1, 512), "fox_forget_b": g("fox_forget_b").reshape(1, 8),
        "rw_mu": g("rw_mu").reshape(1, RWC), "mu_lora": mul,
        "rw_w0": g("rw_w0").reshape(1, 512), "rw_a0": g("rw_a0").reshape(1, 512),
        "rw_decay_up": g("rw_decay_up")[0], "rw_iclr_up": g("rw_iclr_up")[0], "rw_gate_up": g("rw_gate_up")[0],
        "rw_k_k": g("rw_k_k").reshape(1, 512), "rw_k_a": g("rw_k_a").reshape(1, 512),
        "rw_r_k": g("rw_r_k").reshape(1, 512), "rw_lnx_g": g("rw_lnx_g").reshape(1, 512),
        "rw_lnx_b": g("rw_lnx_b").reshape(1, 512),
        "router_w": g("router_w")[0], "router_bias": g("router_bias").reshape(1, NE),
        "exp_w_gate": g("exp_w_gate")[0], "exp_w_up": g("exp_w_up")[0], "exp_w_down": g("exp_w_down")[0],
        "sh_w_gate": g("sh_w_gate")[0], "sh_w_up": g("sh_w_up")[0], "sh_w_down": g("sh_w_down")[0],
    }
    return m


def run(inputs, n_cores, stop=99):
    x = np.asarray(inputs["x"])
    B, T, _ = x.shape
    NB = B // n_cores
    key = (NB, T, stop)
    if key not in _CACHE:
        _CACHE[key] = build(NB, T, stop)
    nc = _CACHE[key]
    in_maps = [_core_inputs(inputs, i * NB, NB) for i in range(n_cores)]
    res = run_bass_kernel_spmd(nc, in_maps, core_ids=list(range(n_cores)))
    return np.concatenate([np.asarray(r["out"]) for r in res.results], axis=0).astype(np.float32)


def kernel(**inputs):
    return run(inputs, 8)
```
